# Optimizing a Trainium2 kernel written in Bass

```python
import math
import jax, jax.numpy as jnp
from jax import lax
import numpy as np

D_MODEL = 1024
BATCH = 8
SEQ = 4096
DEPTH = 1

ATTN_HEADS = 8
ATTN_KV_HEADS = 2
ATTN_HEAD_DIM = 64
ATTN_GROUP = ATTN_HEADS // ATTN_KV_HEADS
WINDOW = 128
ATTN_BLOCK = WINDOW
ATTN_Q_WIDTH = ATTN_HEADS * ATTN_HEAD_DIM
ATTN_KV_WIDTH = ATTN_KV_HEADS * ATTN_HEAD_DIM
MLSTM_HEADS = 4
MLSTM_QK_DIM = 64
MLSTM_V_DIM = 128
MLSTM_QK_WIDTH = MLSTM_HEADS * MLSTM_QK_DIM
MLSTM_V_WIDTH = MLSTM_HEADS * MLSTM_V_DIM
MLSTM_CHUNK = 128
CONV_WIDTH = 4
D_FF = -(-(8 * D_MODEL) // (3 * 256)) * 256
NORM_EPS = 1e-6

SPLIT_SIZES = (ATTN_Q_WIDTH, ATTN_KV_WIDTH, ATTN_KV_WIDTH,
               2 * MLSTM_QK_WIDTH, MLSTM_V_WIDTH,
               MLSTM_V_WIDTH, MLSTM_HEADS, MLSTM_HEADS,
               D_MODEL, D_MODEL)
IN_WIDTH = sum(SPLIT_SIZES)

kernel_name = "hybrid_swa_sink_alibi_mlstm_gated_swiglu"


def _split_points():
    pts, acc = [], 0
    for s in SPLIT_SIZES[:-1]:
        acc += s
        pts.append(acc)
    return pts


def rmsnorm(x, g):
    x32 = x.astype(jnp.float32)
    y = x32 * lax.rsqrt(jnp.mean(x32 * x32, axis=-1, keepdims=True) + NORM_EPS)
    return (y * g.astype(jnp.float32)).astype(x.dtype)


def alibi_slopes(n_heads):
    return jnp.exp2(-8.0 * jnp.arange(1, n_heads + 1, dtype=jnp.float32) / n_heads)


def sliding_window_attention(q, k, v, sinks):
    B, S, _ = q.shape
    W = ATTN_BLOCK
    nb = S // W
    qb = q.reshape(B, nb, W, ATTN_KV_HEADS, ATTN_GROUP, ATTN_HEAD_DIM)
    kb = k.reshape(B, nb, W, ATTN_KV_HEADS, ATTN_HEAD_DIM)
    vb = v.reshape(B, nb, W, ATTN_KV_HEADS, ATTN_HEAD_DIM)

    def with_prev(t):
        prev = jnp.pad(t, ((0, 0), (1, 0), (0, 0), (0, 0), (0, 0)))[:, :-1]
        return jnp.concatenate([prev, t], axis=2)

    kk, vv = with_prev(kb), with_prev(vb)
    s = jnp.einsum('bnqhgd,bnkhd->bhgnqk', qb, kk, preferred_element_type=jnp.float32)
    s = s * (ATTN_HEAD_DIM ** -0.5)
    qi = jnp.arange(W)[:, None]
    kj = jnp.arange(2 * W)[None, :]
    dist = qi - kj + W
    blk = jnp.arange(nb)[:, None, None]
    valid = (dist >= 0) & (dist < WINDOW) & ((blk > 0) | (kj >= W))
    slopes = alibi_slopes(ATTN_HEADS).reshape(ATTN_KV_HEADS, ATTN_GROUP)
    s = s - slopes[:, :, None, None, None] * dist.astype(jnp.float32)
    s = jnp.where(valid, s, -jnp.inf)
    sink = sinks.astype(jnp.float32).reshape(ATTN_KV_HEADS, ATTN_GROUP)[:, :, None, None, None]
    mx = jnp.maximum(jnp.max(s, axis=-1, keepdims=True), sink)
    p = jnp.exp(s - mx)
    p = p / (jnp.sum(p, axis=-1, keepdims=True) + jnp.exp(sink - mx))
    o = jnp.einsum('bhgnqk,bnkhd->bnqhgd', p.astype(v.dtype), vv)
    return o.reshape(B, S, ATTN_Q_WIDTH)


def causal_conv(x, w, b):
    S = x.shape[1]
    xp = jnp.pad(x, ((0, 0), (CONV_WIDTH - 1, 0), (0, 0)))
    out = xp[:, 0:S] * w[0]
    for j in range(1, CONV_WIDTH):
        out = out + xp[:, j:j + S] * w[j]
    return out + b


def mlstm_chunkwise(q, k, v, i_pre, f_pre):
    B, S, H, dk = q.shape
    dv = v.shape[-1]
    L = MLSTM_CHUNK
    nc = S // L

    def chunks(t):
        return t.astype(jnp.float32).reshape(B, nc, L, H, -1).transpose(1, 0, 3, 2, 4)

    def gchunks(t):
        return t.astype(jnp.float32).reshape(B, nc, L, H).transpose(1, 0, 3, 2)

    qs = chunks(q)
    ks = chunks(k) * (dk ** -0.5)
    vs = chunks(v)
    igs = gchunks(i_pre)
    lfs = gchunks(jax.nn.log_sigmoid(f_pre.astype(jnp.float32)))
    causal = jnp.tril(jnp.ones((L, L), dtype=bool))

    def step(carry, xs):
        C, n, m = carry
        qc, kc, vc, ic, lfc = xs
        b = jnp.cumsum(lfc, axis=-1)
        dlog = b[..., :, None] - b[..., None, :] + ic[..., None, :]
        dlog = jnp.where(causal, dlog, -jnp.inf)
        a = b + m[..., None]
        m_t = jnp.maximum(a, jnp.max(dlog, axis=-1))
        w_intra = jnp.exp(dlog - m_t[..., None])
        w_inter = jnp.exp(a - m_t)
        sc = jnp.einsum('bhtd,bhsd->bhts', qc, kc) * w_intra
        num = jnp.einsum('bhts,bhsv->bhtv', sc, vc) + w_inter[..., None] * jnp.einsum('bhtk,bhkv->bhtv', qc, C)
        nq = jnp.sum(sc, axis=-1) + w_inter * jnp.einsum('bhtk,bhk->bht', qc, n)
        h = num / jnp.maximum(jnp.abs(nq), jnp.exp(-m_t))[..., None]
        m_new = m_t[..., -1]
        w_state = jnp.exp(b[..., -1:] - b + ic - m_new[..., None])
        decay = jnp.exp(b[..., -1] + m - m_new)
        C_new = decay[..., None, None] * C + jnp.einsum('bhs,bhsk,bhsv->bhkv', w_state, kc, vc)
        n_new = decay[..., None] * n + jnp.einsum('bhs,bhsk->bhk', w_state, kc)
        return (C_new, n_new, m_new), h

    init = (jnp.zeros((B, H, dk, dv), jnp.float32),
            jnp.zeros((B, H, dk), jnp.float32),
            jnp.zeros((B, H), jnp.float32))
    _, hs = lax.scan(step, init, (qs, ks, vs, igs, lfs))
    return hs.transpose(1, 0, 3, 2, 4).reshape(B, S, H, dv)


def hybrid_layer(x, norm1_g, w_in, conv_w, conv_b, i_bias, f_bias, mlstm_norm_g, attn_sinks,
                 w_attn_branch, w_mlstm_branch, w_out, norm2_g, w_ffn_gate, w_ffn_up, w_ffn_down):
    B, S, _ = x.shape
    u = rmsnorm(x, norm1_g)
    proj = u @ w_in
    aq, ak, av, mqk, mv, mo, mi, mf, ga, gm = jnp.split(proj, _split_points(), axis=-1)

    ya = sliding_window_attention(aq, ak, av, attn_sinks) @ w_attn_branch

    qk = jax.nn.silu(causal_conv(mqk, conv_w, conv_b))
    mq, mk = jnp.split(qk, 2, axis=-1)
    h = mlstm_chunkwise(mq.reshape(B, S, MLSTM_HEADS, MLSTM_QK_DIM),
                        mk.reshape(B, S, MLSTM_HEADS, MLSTM_QK_DIM),
                        mv.reshape(B, S, MLSTM_HEADS, MLSTM_V_DIM),
                        mi + i_bias, mf + f_bias)
    h = rmsnorm(h, mlstm_norm_g.reshape(MLSTM_HEADS, MLSTM_V_DIM)).astype(x.dtype)
    h = h.reshape(B, S, MLSTM_V_WIDTH) * jax.nn.sigmoid(mo)
    ym = h @ w_mlstm_branch

    x = x + (jax.nn.sigmoid(ga) * ya + jax.nn.sigmoid(gm) * ym) @ w_out

    f = rmsnorm(x, norm2_g)
    x = x + (jax.nn.silu(f @ w_ffn_gate) * (f @ w_ffn_up)) @ w_ffn_down
    return x


def setup_inputs(seed: int = 0) -> dict:
    key = jax.random.key(seed)
    ks = jax.random.split(key, 20)
    f32 = jnp.float32

    def nrm(k, shape, scale):
        return jax.random.normal(k, shape, f32) * scale

    return {
        "x": nrm(ks[0], (BATCH, SEQ, D_MODEL), 1.0),
        "norm1_g": 1.0 + nrm(ks[1], (DEPTH, D_MODEL), 0.02),
        "w_in": nrm(ks[2], (DEPTH, D_MODEL, IN_WIDTH), D_MODEL ** -0.5),
        "conv_w": nrm(ks[3], (DEPTH, CONV_WIDTH, 2 * MLSTM_QK_WIDTH), CONV_WIDTH ** -0.5),
        "conv_b": nrm(ks[4], (DEPTH, 2 * MLSTM_QK_WIDTH), 0.01),
        "i_bias": nrm(ks[5], (DEPTH, MLSTM_HEADS), 0.1),
        "f_bias": jnp.linspace(3.0, 6.0, MLSTM_HEADS, dtype=f32)[None, :] + nrm(ks[6], (DEPTH, MLSTM_HEADS), 0.1),
        "mlstm_norm_g": 1.0 + nrm(ks[7], (DEPTH, MLSTM_V_WIDTH), 0.02),
        "attn_sinks": nrm(ks[8], (DEPTH, ATTN_HEADS), 0.5),
        "w_attn_branch": nrm(ks[9], (DEPTH, ATTN_Q_WIDTH, D_MODEL), ATTN_Q_WIDTH ** -0.5),
        "w_mlstm_branch": nrm(ks[10], (DEPTH, MLSTM_V_WIDTH, D_MODEL), MLSTM_V_WIDTH ** -0.5),
        "w_out": nrm(ks[11], (DEPTH, D_MODEL, D_MODEL), D_MODEL ** -0.5),
        "norm2_g": 1.0 + nrm(ks[12], (DEPTH, D_MODEL), 0.02),
        "w_ffn_gate": nrm(ks[13], (DEPTH, D_MODEL, D_FF), D_MODEL ** -0.5),
        "w_ffn_up": nrm(ks[14], (DEPTH, D_MODEL, D_FF), D_MODEL ** -0.5),
        "w_ffn_down": nrm(ks[15], (DEPTH, D_FF, D_MODEL), D_FF ** -0.5),
        "final_norm_g": 1.0 + nrm(ks[16], (D_MODEL,), 0.02),
    }


def reference(x, norm1_g, w_in, conv_w, conv_b, i_bias, f_bias, mlstm_norm_g, attn_sinks,
              w_attn_branch, w_mlstm_branch, w_out, norm2_g, w_ffn_gate, w_ffn_up, w_ffn_down,
              final_norm_g):
    for l in range(DEPTH):
        x = hybrid_layer(x, norm1_g[l], w_in[l], conv_w[l], conv_b[l], i_bias[l], f_bias[l],
                         mlstm_norm_g[l], attn_sinks[l], w_attn_branch[l], w_mlstm_branch[l],
                         w_out[l], norm2_g[l], w_ffn_gate[l], w_ffn_up[l], w_ffn_down[l])
    return rmsnorm(x, final_norm_g)
```

```python
import math
from contextlib import ExitStack

import numpy as np
import concourse.bass as bass
import concourse.mybir as mybir
from concourse.bass_utils import run_bass_kernel_spmd

F32 = mybir.dt.float32
BF16 = mybir.dt.bfloat16
AF = mybir.ActivationFunctionType
ALU = mybir.AluOpType
AX = mybir.AxisListType

S = 4096
D = 1024
DFF = 2816
NDC = DFF // 128
INW = 4360
EPS = 1e-6
ST = 256
NT = ST // 128
NS = S // ST
GT = 512
NGT = GT // 128
NG = S // GT
NTILES = S // 128

C_Q = 0
C_K = 512
C_MQK = 640
C_GA = 1152
C_GM = 2176
C_MV = 3200
C_MO = 3712
C_T3 = 4224

K_IDF = 0
K_TRI = 128
K_G1 = 256
K_GM = 1280
K_B8 = 1792
K_CW = 1800
K_CB = 1816
K_ONE = 1820
K_END = 1948
KB_E = 0
KB_ONA = 2048
KB_IDB = 2304
KB_END = 2432

ENGS = ("pe", "act", "dve", "pool", "sp")
SYNC_EACH = True


class Prog:
    def __init__(self, nc, stack):
        self.nc = nc
        self.stack = stack
        self.ops = {e: [] for e in ENGS}
        self.esem = {e: stack.enter_context(nc.semaphore("s_" + e)) for e in ENGS}
        self.ecnt = {e: 0 for e in ENGS}
        self.known = {e: {} for e in ENGS}
        self.semobj = {}
        self.last_w = {}
        self.readers = {}
        self.dsem_cnt = {}

    def dma_sem(self, name):
        s = self.stack.enter_context(self.nc.semaphore(name))
        self.dsem_cnt[name] = 0
        self.semobj[name] = s
        return name

    def _waits(self, eng, reads, writes):
        need = {}

        def add(tok):
            if tok is None:
                return
            sname, val = tok
            if val > need.get(sname, 0):
                need[sname] = val

        for k in reads:
            add(self.last_w.get(k))
        for k in writes:
            add(self.last_w.get(k))
            for t in self.readers.get(k, ()):
                add(t)
        out = []
        kn = self.known[eng]
        for sname, val in need.items():
            if kn.get(sname, 0) >= val:
                continue
            kn[sname] = val
            out.append((sname, val))
        return out

    def _commit(self, tok, reads, writes):
        for k in reads:
            lst = self.readers.setdefault(k, {})
            lst[tok[0]] = max(lst.get(tok[0], 0), tok[1])
        for k in writes:
            self.last_w[k] = tok
            self.readers[k] = {}

    def _waits2(self, eng, reads, writes):
        return self._waits(eng, reads, writes)

    def op(self, eng, fn, reads=(), writes=()):
        waits = self._waits(eng, reads, writes)
        if eng == "pe" and getattr(self, "_pe_drain", 0) > self.known["pe"].get("s_pe", 0):
            self.known["pe"]["s_pe"] = self._pe_drain
            waits.append(("s_pe", self._pe_drain))
        self.ecnt[eng] += 1
        tok = ("s_" + eng, self.ecnt[eng])
        self.ops[eng].append((waits, fn, ("s_" + eng, 1)))
        self._commit(tok, reads, writes)
        return tok

    def dma(self, eng, sem, out, in_, reads=(), writes=()):
        if sem is None:
            self._nsem = getattr(self, "_nsem", 0) + 1
            sem = self.dma_sem("d_one%d" % self._nsem)
        waits = self._waits(eng, reads, writes)
        prev = self.dsem_cnt[sem]
        if prev > 0 and self.known[eng].get(sem, 0) < prev:
            self.known[eng][sem] = prev
            waits.append((sem, prev))
        self.dsem_cnt[sem] += 16
        tok = (sem, self.dsem_cnt[sem])
        self.ops[eng].append((waits, lambda e, o=out, i=in_: e.dma_start(out=o, in_=i), (sem, 16)))
        self._commit(tok, reads, writes)
        return tok

    def barrier(self):
        allv = {("s_" + e): self.ecnt[e] for e in ENGS}
        allv.update(self.dsem_cnt)
        for e in ENGS:
            waits = []
            for sname, val in allv.items():
                if val > 0 and self.known[e].get(sname, 0) < val:
                    self.known[e][sname] = val
                    waits.append((sname, val))
            self.ops[e].append((waits, None, None))

    def _sem(self, name):
        if name in self.semobj:
            return self.semobj[name]
        return self.esem[name[2:]]

    def emit(self):
        nc = self.nc
        with nc.Block() as block:
            def run(e, name):
                for waits, fn, inc in self.ops[name]:
                    for sname, val in waits:
                        e.wait_ge(self._sem(sname), val)
                    if fn is None:
                        continue
                    ins = fn(e)
                    ins.then_inc(self._sem(inc[0]), inc[1])

            @block.tensor
            def _(e):
                run(e, "pe")

            @block.scalar
            def _(e):
                run(e, "act")

            @block.vector
            def _(e):
                run(e, "dve")

            @block.gpsimd
            def _(e):
                run(e, "pool")

            @block.sync
            def _(e):
                run(e, "sp")

    def _iter_readers(self, k):
        return self.readers.get(k, {}).items()


def _patched_waits(self, eng, reads, writes):
    need = {}

    def add(sname, val):
        if val > need.get(sname, 0):
            need[sname] = val

    for k in reads:
        t = self.last_w.get(k)
        if t is not None:
            add(*t)
    for k in writes:
        t = self.last_w.get(k)
        if t is not None:
            add(*t)
        for sname, val in self.readers.get(k, {}).items():
            add(sname, val)
    out = []
    kn = self.known[eng]
    for sname, val in need.items():
        if kn.get(sname, 0) >= val:
            continue
        kn[sname] = val
        out.append((sname, val))
    return out


Prog._waits = _patched_waits


class Arena:
    def __init__(self, t, nbytes):
        self.t = t
        self.nbytes = nbytes
        self.off = 0
        self.peak = 0

    def alloc(self, shape, dt):
        n = 1
        for s_ in shape:
            n *= s_
        esz = 4 if dt == F32 else 2
        self.off = (self.off + 31) // 32 * 32
        b0 = self.off
        self.off += n * esz
        self.peak = max(self.peak, self.off)
        assert self.off <= self.nbytes, ("SBUF arena overflow", self.off, self.nbytes)
        v = self.t[:, b0 // 2:(b0 + n * esz) // 2]
        if dt == F32:
            v = v.bitcast(F32)
        if len(shape) == 2:
            v = v.rearrange("p (a b) -> p a b", a=shape[0])
        elif len(shape) == 3:
            v = v.rearrange("p (a b c) -> p a b c", a=shape[0], b=shape[1])
        elif len(shape) == 4:
            v = v.rearrange("p (a b c d) -> p a b c d", a=shape[0], b=shape[1], c=shape[2])
        return v


def cap(v, off, dims, nparts=128, pstart=0):
    ps = v.ap[0][0]
    return bass.AP(v.tensor, v.offset + pstart * ps + off, [[ps, nparts]] + [list(d) for d in dims])


def rel(v, sub):
    return sub.offset - v.offset


class _StopBuild(Exception):
    pass


def build_program(ns=NS, ng=NG, lim=99, lim_s=0):
    nc = bass.Bass("TRN2", target_bir_lowering=False)

    def din(name, shape):
        return nc.dram_tensor(name, list(shape), F32, kind="ExternalInput").ap()

    x_d = din("x", [S, D])
    win_d = din("w_in", [D, INW])
    wab_d = din("w_ab", [512, D])
    wmb_d = din("w_mb", [512, D])
    wout_d = din("w_out", [D, D])
    wg_d = din("w_g", [D, DFF])
    wu_d = din("w_u", [D, DFF])
    wd_d = din("w_d", [DFF, D])
    cst_d = din("cst", [128, K_END])
    cstb_d = din("cstb", [128, KB_END])
    snk_d = din("snk", [128, 512])
    cst2_d = din("cst2", [128, 2048])
    out_d = nc.dram_tensor("out", [S, D], F32, kind="ExternalOutput").ap()
    x1s_d = nc.dram_tensor("x1_scratch", [S, D], F32, kind="Internal").ap()

    NBYTES = 212000
    with ExitStack() as st:
        P = Prog(nc, st)
        arena_t = st.enter_context(nc.sbuf_tensor("arena", [128, NBYTES // 2], BF16))
        A = Arena(arena_t, NBYTES)
        pbank = [st.enter_context(nc.psum_tensor("pb%d" % i, [128, 512], F32))[:] for i in range(7)]
        ptr_t = st.enter_context(nc.psum_tensor("ptr", [128, 1024], BF16))[:]

        def act(out, in_, func, reads, writes, bias=None, scale=None, accum=None):
            kw = {}
            if bias is not None:
                kw["bias"] = bias
            if scale is not None:
                kw["scale"] = scale
            if accum is not None:
                kw["accum_out"] = accum
            return P.op("act", lambda e: e.activation(out, in_, func, **kw), reads, writes)

        def tt(eng, out, in0, in1, op, reads, writes):
            return P.op(eng, lambda e: e.tensor_tensor(out, in0, in1, op), reads, writes)

        def ts(out, in0, s1, s2, op0, op1, reads, writes, eng="dve"):
            if s2 is None:
                return P.op(eng, lambda e: e.tensor_scalar(out, in0, s1, None, op0), reads, writes)
            return P.op(eng, lambda e: e.tensor_scalar(out, in0, s1, s2, op0, op1), reads, writes)

        def stt(out, in0, scalar, in1, op0, op1, reads, writes, eng="dve"):
            return P.op(eng, lambda e: e.scalar_tensor_tensor(out, in0, scalar, in1, op0, op1), reads, writes)

        def cpy(eng, out, in_, reads, writes):
            if eng == "act":
                return P.op("act", lambda e: e.copy(out, in_), reads, writes)
            return P.op(eng, lambda e: e.tensor_copy(out, in_), reads, writes)

        def mm(out, pairs, reads, writes):
            def fn(e):
                n = len(pairs)
                ins = None
                for i, (l, r) in enumerate(pairs):
                    ins = e.matmul(out, l, r, start=(i == 0), stop=(i == n - 1))
                return ins
            return P.op("pe", fn, reads, writes)

        def mm_multi(groups, reads, writes):
            def fn(e):
                ins = None
                for out, pairs in groups:
                    n = len(pairs)
                    for i, (l, r) in enumerate(pairs):
                        ins = e.matmul(out, l, r, start=(i == 0), stop=(i == n - 1))
                return ins
            return P.op("pe", fn, reads, writes)

        def transposes(items, ident, reads, writes):
            def fn(e):
                ins = None
                for o, i_ in items:
                    ins = e.transpose(o, i_, ident)
                return ins
            tok = P.op("pe", fn, reads, writes)
            P._pe_drain = tok[1]
            return tok

        def sigmoid_inplace(dst, src, reads_src, key, neg_bias=None):
            if neg_bias is None:
                act(dst, src, AF.Exp, reads_src, [key], scale=-1.0)
            else:
                act(dst, src, AF.Exp, reads_src, [key], scale=-1.0, bias=neg_bias)
            act(dst, dst, AF.Ln, [key], [key], bias=1.0)
            act(dst, dst, AF.Exp, [key], [key], scale=-1.0)

        cst2 = A.alloc([2048], F32)
        identb = A.alloc([128], BF16)
        g2rep = cst2[:, 0:1024]
        gfrep = cst2[:, 1024:2048]
        base_off = A.off
        cst = A.alloc([K_END], F32)
        cstb = A.alloc([KB_IDB], BF16)
        sinkT = A.alloc([512], F32)
        nconvb = A.alloc([4], F32)
        identf = cst[:, K_IDF:K_IDF + 128]
        triu = cst[:, K_TRI:K_TRI + 128]
        g1rep = cst[:, K_G1:K_G1 + 1024]
        gmrep = cst[:, K_GM:K_GM + 512]
        bias8 = cst[:, K_B8:K_B8 + 8]
        convw = cst[:, K_CW:K_CW + 16]
        convb = cst[:, K_CB:K_CB + 4]
        onesf = cst[:, K_ONE:K_ONE + 128]
        Etab = cstb[:, KB_E:KB_E + 2048]
        onesA = cstb[:, KB_ONA:KB_ONA + 256]

        d_c = P.dma_sem("d_cst")
        P.dma("sp", None, cst, cst_d, writes=["cst"])
        P.dma("sp", None, sinkT, snk_d, writes=["sinkT"])
        P.dma("sp", None, cst2, cst2_d, writes=["cst2"])
        d_cb = P.dma_sem("d_cstb")
        P.dma("pool", None, cstb, cstb_d[:, 0:KB_IDB], writes=["cstb"])
        P.dma("pool", None, identb, cstb_d[:, KB_IDB:KB_END], writes=["identb"])
        act(sinkT, sinkT, AF.Exp, ["sinkT"], ["sinkT"])
        ts(nconvb, convb, -1.0, None, ALU.mult, None, ["cst"], ["nconvb"])

        Wi = A.alloc([8, INW], BF16)
        Wab = A.alloc([4, D], BF16)
        Wmb = A.alloc([4, D], BF16)
        Wout = A.alloc([8, D], BF16)
        KT = A.alloc([2, ST], BF16)
        VA = A.alloc([8, 2, 128], BF16)
        xt = [A.alloc([D], F32) for _ in range(2)]
        xn = [A.alloc([D], BF16) for _ in range(2)]
        uT = A.alloc([8, ST], BF16)
        QT = A.alloc([4, ST], BF16)
        rawqk = A.alloc([4, ST + 3], F32)
        acc = A.alloc([ST], F32)
        sgq = A.alloc([ST], F32)
        qkT = A.alloc([4, ST], BF16)
        VT = [A.alloc([4, 136], BF16) for _ in range(NT)]
        gsig = [A.alloc([512], F32) for _ in range(NT)]
        praw = [A.alloc([512], BF16) for _ in range(2)]
        pt = [A.alloc([512], BF16) for _ in range(4)]
        rden = A.alloc([512], F32)
        AOT = A.alloc([4, ST], BF16)
        ktokA = A.alloc([4, 128], BF16)
        scm = A.alloc([4, 128], BF16)
        Cst = A.alloc([2, 136], F32)
        Chat = A.alloc([2, 136], F32)
        Cbf = A.alloc([2, 2, 136], BF16)
        hg = A.alloc([512], BF16)
        hgT = A.alloc([4, ST], BF16)
        sga = A.alloc([ST], F32)
        sgm = A.alloc([ST], F32)
        mT = A.alloc([8, ST], BF16)
        xr = [A.alloc([D], F32) for _ in range(2)]
        junk = A.alloc([D], BF16)
        ss1 = A.alloc([NT], F32)
        rstd1 = A.alloc([NT], F32)
        gif = A.alloc([NT, 8], F32)
        l4 = A.alloc([NT, 4], F32)
        gn = A.alloc([NT, 8], F32)
        gmax = A.alloc([NT], F32)
        nbL = A.alloc([NT], F32)
        Rr = A.alloc([NT], F32)
        Mall = A.alloc([NT + 1], F32)
        darg = A.alloc([NT], F32)
        dec = A.alloc([NT], F32)
        diag = A.alloc([NT, 8], F32)
        bcs = A.alloc([NT, 8], F32)
        garg = A.alloc([NT, 8], F32)
        ex = A.alloc([NT, 8], F32)
        decsel = A.alloc([NT, 2], F32)
        absn = A.alloc([4], F32)
        den4 = A.alloc([4], F32)
        rden4 = A.alloc([4], F32)
        ssh = A.alloc([4], F32)
        rsh = A.alloc([4], F32)
        sc2 = A.alloc([4], F32)
        print("phase A arena bytes:", A.off)

        d_w = P.dma_sem("d_wA")
        win_v = win_d.rearrange("(k p) n -> p k n", p=128)
        NSL = 8
        slab = INW // NSL
        for i in range(NSL):
            P.dma("pool", None, Wi[:, :, i * slab:(i + 1) * slab], win_v[:, :, i * slab:(i + 1) * slab], writes=["Wi"])
        P.dma("pool", None, Wab, wab_d.rearrange("(k p) n -> p k n", p=128), writes=["Wab"])
        P.dma("pool", None, Wmb, wmb_d.rearrange("(k p) n -> p k n", p=128), writes=["Wmb"])
        P.dma("pool", None, Wout, wout_d.rearrange("(k p) n -> p k n", p=128), writes=["Wout"])

        P.op("pool", lambda e: e.memset(VA.rearrange("p a b c -> p (a b c)"), 0.0), writes=["VA%d" % i for i in range(8)])
        P.op("pool", lambda e: e.memset(ktokA.rearrange("p a b -> p (a b)"), 0.0), writes=["ktokA"])
        P.op("pool", lambda e: e.memset(Cst.rearrange("p a b -> p (a b)"), 0.0), writes=["Cst"])
        P.op("pool", lambda e: e.memset(Mall, 0.0), writes=["Mall"])
        P.op("pool", lambda e: e.memset(Cbf.rearrange("p a b c -> p (a b c)"), 0.0), writes=["Cbf"])
        for c_ in range(NT):
            P.op("pool", lambda e, c_=c_: e.memset(VT[c_].rearrange("p a b -> p (a b)"), 0.0), writes=["VT%d" % c_])
        P.op("pool", lambda e: e.memset(rawqk[:, :, 0:3], 0.0), writes=["rawqk%d" % j for j in range(4)])

        d_x = [P.dma_sem("d_x%d" % i) for i in range(2)]
        d_xr = [P.dma_sem("d_xr%d" % i) for i in range(2)]
        d_o = [P.dma_sem("d_o%d" % i) for i in range(2)]

        pmm = [pbank[0], pbank[1]]
        pmm_i = [0]

        def next_pmm():
            i = pmm_i[0]
            pmm_i[0] ^= 1
            return pmm[i], "pmm%d" % i

        psc = [pbank[2], pbank[3]]
        pO, pD = pbank[4], pbank[5]
        psmall = pbank[6]

        try:
          for s in range(ns):
            kr = s % 2
            if s > 0 and SYNC_EACH:
                P.barrier()
            pmm_i[0] = 0
            for c in range(NT):
                i = s * NT + c
                sl = i % 2
                P.dma("sp", d_x[sl], xt[sl], x_d[i * 128:(i + 1) * 128, :], writes=["xt%d" % sl])
                act(junk, xt[sl], AF.Square, ["xt%d" % sl], ["junk", "ss1_%d" % c], accum=ss1[:, c:c + 1])
                act(rstd1[:, c:c + 1], ss1[:, c:c + 1], AF.Ln, ["ss1_%d" % c], ["rstd1_%d" % c], scale=1.0 / D, bias=EPS)
                act(rstd1[:, c:c + 1], rstd1[:, c:c + 1], AF.Exp, ["rstd1_%d" % c], ["rstd1_%d" % c], scale=-0.5)
                stt(xn[sl], xt[sl], rstd1[:, c:c + 1], g1rep, ALU.mult, ALU.mult,
                    ["xt%d" % sl, "rstd1_%d" % c, "cst"], ["xn%d" % sl])
                transposes([(ptr_t[:, k * 128:(k + 1) * 128], xn[sl][:, k * 128:(k + 1) * 128]) for k in range(8)],
                           identb, ["xn%d" % sl, "identb"], ["ptr"])
                cpy("act", uT[:, :, c * 128:(c + 1) * 128], ptr_t.rearrange("p (k t) -> p k t", k=8),
                    ["ptr"], ["uT%d" % c])
            uTkeys = ["uT%d" % c for c in range(NT)]

            if s >= lim_s and lim < 3:
                raise _StopBuild()
            for c in range(NT):
                i = s * NT + c
                vr = i % 8
                pb, pk = next_pmm()
                mm(pb[:, 0:136], [(uT[:, k, c * 128:(c + 1) * 128], Wi[:, k, C_T3:C_T3 + 136]) for k in range(8)],
                   ["uT%d" % c, "Wi"], [pk])
                cpy("act", cap(VA, rel(VA, VA[:, vr]), [[192, 2], [1, 64]]),
                    pb[:, 0:128].rearrange("p (g d) -> p g d", g=2), [pk], ["VA%d" % vr, "s3ord"])
                tt("dve", gif[:, c, :], pb[:, 128:136], bias8, ALU.add, [pk, "cst", "s3ord"], ["gif"])
            if s >= lim_s and lim < 4:
                raise _StopBuild()
            act(l4, gif[:, :, 4:8], AF.Exp, ["gif"], ["l4"], scale=-1.0)
            act(l4, l4, AF.Ln, ["l4"], ["l4"], bias=1.0)
            mm(psmall[:, 0:4 * NT], [(triu, l4.rearrange("p c h -> p (c h)"))], ["cst", "l4"], ["psmall"])
            pg1v = psmall[:, 0:4 * NT].rearrange("p (c h) -> p c h", c=NT)
            tt("dve", gn[:, :, 0:4], gif[:, :, 0:4], pg1v, ALU.add, ["gif", "psmall"], ["gn"])
            cpy("dve", gn[:, :, 4:8], pg1v, ["psmall"], ["gn"])
            G2 = 64
            groups = []
            for c in range(NT):
                groups.append((psmall[0:4, G2 + c * 128:G2 + (c + 1) * 128], [(gn[:, c, 0:4], identf)]))
            for c in range(NT):
                groups.append((psmall[0:4, G2 + NT * 128 + c:G2 + NT * 128 + c + 1], [(l4[:, c, :], onesf[:, 0:1])]))
            mm_multi(groups, ["gn", "l4", "cst"], ["psmall"])
            P.op("dve", lambda e: e.reduce_max(gmax[0:4, :], psmall[0:4, G2:G2 + NT * 128].rearrange("p (c t) -> p c t", c=NT), AX.X),
                 ["psmall"], ["gmax"])
            cpy("dve", nbL[0:4, :], psmall[0:4, G2 + NT * 128:G2 + NT * 128 + NT], ["psmall"], ["nbL"])
            for c in range(NT):
                tt("dve", Rr[0:4, c:c + 1], gmax[0:4, c:c + 1], Mall[0:4, c:c + 1], ALU.max, ["gmax", "Mall"], ["Rr"])
                tt("dve", Mall[0:4, c + 1:c + 2], Rr[0:4, c:c + 1], nbL[0:4, c:c + 1], ALU.subtract, ["Rr", "nbL"], ["Mall"])
            tt("dve", darg[0:4, :], Mall[0:4, 0:NT], Rr[0:4, :], ALU.subtract, ["Mall", "Rr"], ["darg"])
            act(dec[0:4, :], darg[0:4, :], AF.Exp, ["darg"], ["dec"])
            cpy("dve", Mall[0:4, 0:1], Mall[0:4, NT:NT + 1], ["Mall"], ["Mall"])
            I4b = cap(cst, K_IDF, [[0, NT], [1, 4]], nparts=4)
            tt("dve", diag[0:4, :, 0:4], I4b, cap(Rr, 0, [[1, NT], [0, 4]], nparts=4), ALU.mult, ["cst", "Rr"], ["diag"])
            tt("dve", diag[0:4, :, 4:8], I4b, cap(dec, 0, [[1, NT], [0, 4]], nparts=4), ALU.mult, ["cst", "dec"], ["diag"])
            G3 = 448
            mm(psmall[:, G3:G3 + 8 * NT], [(onesf[0:4, :], diag[0:4].rearrange("p c h -> p (c h)"))], ["cst", "diag"], ["psmall"])
            cpy("dve", bcs, psmall[:, G3:G3 + 8 * NT].rearrange("p (c h) -> p c h", c=NT), ["psmall"], ["bcs"])
            tt("dve", garg.rearrange("p c (a h) -> p c a h", a=2), gn.rearrange("p c (a h) -> p c a h", a=2),
               cap(bcs, 0, [[8, NT], [0, 2], [1, 4]]), ALU.subtract, ["gn", "bcs"], ["garg"])
            act(ex, garg, AF.Exp, ["garg"], ["ex"])
            cpy("dve", decsel[0:64], cap(bcs, 4, [[8, NT], [2, 2]], nparts=64), ["bcs"], ["decsel"])
            cpy("dve", decsel[64:128], cap(bcs, 5, [[8, NT], [2, 2]], nparts=64, pstart=64), ["bcs"], ["decsel"])

            if s >= lim_s and lim < 5:
                raise _StopBuild()
            for j in range(4):
                pb, pk = next_pmm()
                mm(pb[:, 0:ST], [(Wi[:, k, C_Q + j * 128:C_Q + (j + 1) * 128], uT[:, k, :]) for k in range(8)],
                   uTkeys + ["Wi"], [pk])
                cpy("act", QT[:, j, :], pb[:, 0:ST], [pk], ["QT"])
            pb, pk = next_pmm()
            mm(pb[:, 0:ST], [(Wi[:, k, C_K:C_K + 128], uT[:, k, :]) for k in range(8)], uTkeys + ["Wi"], [pk])
            cpy("act", KT[:, kr, :], pb[:, 0:ST], [pk], ["KT%d" % kr])
            for j in range(4):
                pb, pk = next_pmm()
                mm(pb[:, 0:ST], [(Wi[:, k, C_MQK + j * 128:C_MQK + (j + 1) * 128], uT[:, k, :]) for k in range(8)],
                   uTkeys + ["Wi"], [pk])
                rk = "rawqk%d" % j
                cpy("act", rawqk[:, j, 3:3 + ST], pb[:, 0:ST], [pk], [rk])
                ts(acc, rawqk[:, j, 0:ST], convw[:, j * 4:j * 4 + 1], None, ALU.mult, None, [rk, "cst"], ["acc"])
                for tap in range(1, 4):
                    stt(acc, rawqk[:, j, tap:tap + ST], convw[:, j * 4 + tap:j * 4 + tap + 1], acc, ALU.mult, ALU.add,
                        [rk, "acc", "cst"], ["acc"])
                cpy("dve", rawqk[:, j, 0:3], rawqk[:, j, ST:ST + 3], [rk], [rk])
                sigmoid_inplace(sgq, acc, ["acc", "nconvb"], "sgq", neg_bias=nconvb[:, j:j + 1])
                stt(qkT[:, j, :], acc, convb[:, j:j + 1], sgq, ALU.add, ALU.mult, ["acc", "sgq", "cst"], ["qkT%d" % j])

            if s >= lim_s and lim < 6:
                raise _StopBuild()
            for c in range(NT):
                pb, pk = next_pmm()
                mm(pb, [(uT[:, k, c * 128:(c + 1) * 128], Wi[:, k, C_MV:C_MV + 512]) for k in range(8)],
                   ["uT%d" % c, "Wi"], [pk])
                stt(VT[c][:, :, 0:128], pb.rearrange("p (h v) -> p h v", h=4), 0.125,
                    cap(ex, c * 8, [[1, 4], [0, 128]]), ALU.mult, ALU.mult, [pk, "ex"], ["VT%d" % c])
                ts(VT[c][:, :, 128], ex[:, c, 0:4], 0.125, None, ALU.mult, None, ["ex"], ["VT%d" % c])
                pb, pk = next_pmm()
                mm(pb, [(uT[:, k, c * 128:(c + 1) * 128], Wi[:, k, C_MO:C_MO + 512]) for k in range(8)],
                   ["uT%d" % c, "Wi"], [pk])
                sigmoid_inplace(gsig[c], pb, [pk], "gsig%d" % c)
                tt("dve", gsig[c], gsig[c], gmrep, ALU.mult, ["gsig%d" % c, "cst"], ["gsig%d" % c])

            if s >= lim_s and lim < 7:
                raise _StopBuild()
            for c in range(NT):
                i = s * NT + c
                tsl = slice(c * 128, (c + 1) * 128)
                blocks = []
                if i > 0:
                    pi_ = i - 1
                    blocks.append((0, (pi_ // NT) % 2, (pi_ % NT) * 128, pi_ % 8))
                blocks.append((1, kr, c * 128, i % 8))
                pts = []
                n = 0
                for g in range(2):
                    for (b, krr, koff, vr) in blocks:
                        ps_ = psc[n % 2]
                        psk = "psc%d" % (n % 2)
                        mm(ps_, [(KT[g * 64:(g + 1) * 64, krr, koff:koff + 128],
                                  QT[g * 64:(g + 1) * 64, :, tsl])], ["KT%d" % krr, "QT"], [psk])
                        pr = praw[n % 2]
                        prk = "praw%d" % (n % 2)
                        act(pr, ps_, AF.Exp, [psk], [prk], scale=0.125)
                        tt("dve", pt[n], pr, Etab[:, (b * 2 + g) * 512:(b * 2 + g + 1) * 512], ALU.mult,
                           [prk, "cstb"], ["pt%d" % n])
                        pts.append((n, g, vr))
                        n += 1
                mm(pO, [(VA[:, vr, g, :], pt[n_]) for (n_, g, vr) in pts],
                   ["pt%d" % n_ for (n_, _, _) in pts] + ["VA%d" % vr for (_, _, vr) in pts], ["pO"])
                dpairs = [(onesA[:, g * 128:(g + 1) * 128], pt[n_]) for (n_, g, vr) in pts]
                mm(pD, dpairs, ["pt%d" % n_ for (n_, _, _) in pts] + ["cstb"], ["pD"])
                tt("dve", rden, pD, sinkT, ALU.add, ["pD", "sinkT"], ["rden"])
                P.op("dve", lambda e: e.reciprocal(rden, rden), ["rden"], ["rden"])
                tt("dve", AOT[:, :, tsl], pO.rearrange("p (j q) -> p j q", j=4), rden.rearrange("p (j q) -> p j q", j=4),
                   ALU.mult, ["pO", "rden"], ["AOT%d" % c])

                if s >= lim_s and lim <= 7.0:
                    raise _StopBuild()
                transposes([(ptr_t[:, 0:128], qkT[:, 2, tsl]), (ptr_t[:, 128:256], qkT[:, 3, tsl])], identb,
                           ["qkT2", "qkT3", "identb"], ["ptr"])
                for h in range(4):
                    cpy("act", ktokA[:, h, (h % 2) * 64:(h % 2) * 64 + 64], ptr_t[:, h * 64:(h + 1) * 64], ["ptr"], ["ktokA"])
                if s >= lim_s and lim < 7.1:
                    raise _StopBuild()
                mm_multi([(psc[h % 2][:, (h // 2) * 128:(h // 2 + 1) * 128],
                           [(qkT[(h % 2) * 64:(h % 2) * 64 + 64, 2 + h // 2, tsl],
                             qkT[(h % 2) * 64:(h % 2) * 64 + 64, h // 2, tsl])]) for h in range(4)],
                         ["qkT%d" % j for j in range(4)], ["psc0", "psc1"])
                for e_ in range(2):
                    tt("dve", cap(scm, e_ * 128, [[256, 2], [1, 128]]),
                       psc[e_][:, 0:256].rearrange("p (h t) -> p h t", h=2), cap(cst, K_TRI, [[0, 2], [1, 128]]), ALU.mult,
                       ["psc%d" % e_, "cst"], ["scm"])
                if s >= lim_s and lim < 7.3:
                    raise _StopBuild()
                for p_ in range(2):
                    ts(Chat[:, p_, :], Cst[:, p_, :], decsel[:, c, p_:p_ + 1], None, ALU.mult, None,
                       ["Cst", "decsel"], ["Chat"])
                for e_ in range(2):
                    cpy("act", Cbf[e_ * 64:(e_ + 1) * 64, :, e_, :], Chat[e_ * 64:(e_ + 1) * 64, :, :], ["Chat"], ["Cbf"])
                pin = [psc[1], pO]
                pink = ["psc1", "pO"]
                for p_ in range(2):
                    groups = []
                    for e_ in range(2):
                        h = 2 * p_ + e_
                        groups.append((pin[p_][:, e_ * 136:e_ * 136 + 136],
                                       [(scm[:, h, :], VT[c][:, h, :]),
                                        (qkT[:, p_, tsl], Cbf[:, p_, e_, :])]))
                    mm_multi(groups, ["scm", "VT%d" % c, "qkT0", "qkT1", "Cbf"], [pink[p_]])
                if s >= lim_s and lim < 7.5:
                    raise _StopBuild()
                pdc = pD
                mm_multi([(pdc[:, p_ * 136:p_ * 136 + 136],
                           [(ktokA[:, 2 * p_, :], VT[c][:, 2 * p_, :]), (ktokA[:, 2 * p_ + 1, :], VT[c][:, 2 * p_ + 1, :])])
                          for p_ in range(2)], ["ktokA", "VT%d" % c], ["pD"])
                tt("dve", Cst, Chat, pdc[:, 0:272].rearrange("p (a v) -> p a v", a=2), ALU.add, ["Chat", "pD"], ["Cst"])
                if s >= lim_s and lim < 7.7:
                    raise _StopBuild()
                for p_ in range(2):
                    ts(absn[:, 2 * p_:2 * p_ + 2], cap(pin[p_], 128, [[136, 2]]), -1.0, None, ALU.mult, None, [pink[p_]], ["absn"])
                    tt("dve", absn[:, 2 * p_:2 * p_ + 2], absn[:, 2 * p_:2 * p_ + 2], cap(pin[p_], 128, [[136, 2]]), ALU.max,
                       ["absn", pink[p_]], ["absn"])
                tt("dve", den4, absn, ex[:, c, 4:8], ALU.max, ["absn", "ex"], ["den4"])
                P.op("dve", lambda e: e.reciprocal(rden4, den4), ["den4"], ["rden4"])
                for h in range(4):
                    act(junk[:, 0:128], pin[h // 2][:, (h % 2) * 136:(h % 2) * 136 + 128], AF.Square,
                        [pink[h // 2], "den4"], ["junk", "ssh"], accum=ssh[:, h:h + 1])
                tt("dve", ssh, ssh, rden4, ALU.mult, ["ssh", "rden4"], ["ssh"])
                tt("dve", ssh, ssh, rden4, ALU.mult, ["ssh", "rden4"], ["ssh"])
                act(rsh, ssh, AF.Ln, ["ssh"], ["rsh"], scale=1.0 / 128, bias=EPS)
                act(rsh, rsh, AF.Exp, ["rsh"], ["rsh"], scale=-0.5)
                tt("dve", sc2, rden4, rsh, ALU.mult, ["rden4", "rsh"], ["sc2"])
                if s >= lim_s and lim < 7.9:
                    raise _StopBuild()
                for h in range(4):
                    stt(hg[:, h * 128:(h + 1) * 128], pin[h // 2][:, (h % 2) * 136:(h % 2) * 136 + 128],
                        sc2[:, h:h + 1], gsig[c][:, h * 128:(h + 1) * 128], ALU.mult, ALU.mult,
                        [pink[h // 2], "sc2", "gsig%d" % c], ["hg"])
                transposes([(ptr_t[:, h * 128:(h + 1) * 128], hg[:, h * 128:(h + 1) * 128]) for h in range(4)], identb,
                           ["hg", "identb"], ["ptr"])
                cpy("act", hgT[:, :, tsl], ptr_t[:, 0:512].rearrange("p (h t) -> p h t", h=4), ["ptr"], ["hgT%d" % c])
                if s >= lim_s and lim < 7.95:
                    raise _StopBuild()

            if s >= lim_s and lim < 9:
                raise _StopBuild()
            aot_keys = ["AOT%d" % c for c in range(NT)]
            hgt_keys = ["hgT%d" % c for c in range(NT)]
            for f in range(8):
                pb, pk = next_pmm()
                mm(pb[:, 0:ST], [(Wi[:, k, C_GA + f * 128:C_GA + (f + 1) * 128], uT[:, k, :]) for k in range(8)],
                   uTkeys + ["Wi"], [pk])
                sigmoid_inplace(sga, pb[:, 0:ST], [pk], "sga")
                pb, pk = next_pmm()
                mm(pb[:, 0:ST], [(Wi[:, k, C_GM + f * 128:C_GM + (f + 1) * 128], uT[:, k, :]) for k in range(8)],
                   uTkeys + ["Wi"], [pk])
                sigmoid_inplace(sgm, pb[:, 0:ST], [pk], "sgm")
                pb, pk = next_pmm()
                mm(pb[:, 0:ST], [(Wab[:, j, f * 128:(f + 1) * 128], AOT[:, j, :]) for j in range(4)],
                   aot_keys + ["Wab"], [pk])
                tt("dve", sga, sga, pb[:, 0:ST], ALU.mult, ["sga", pk], ["sga"])
                pb, pk = next_pmm()
                mm(pb[:, 0:ST], [(Wmb[:, h, f * 128:(f + 1) * 128], hgT[:, h, :]) for h in range(4)],
                   hgt_keys + ["Wmb"], [pk])
                tt("dve", sgm, sgm, pb[:, 0:ST], ALU.mult, ["sgm", pk], ["sgm"])
                tt("pool", mT[:, f, :], sga, sgm, ALU.add, ["sga", "sgm"], ["mT"])

            if s >= lim_s and lim < 10:
                raise _StopBuild()
            for c in range(NT):
                i = s * NT + c
                sl = i % 2
                P.dma("sp", d_xr[sl], xr[sl], x_d[i * 128:(i + 1) * 128, :], writes=["xr%d" % sl])
                for hf in range(2):
                    pb, pk = next_pmm()
                    mm(pb, [(mT[:, k, c * 128:(c + 1) * 128], Wout[:, k, hf * 512:(hf + 1) * 512]) for k in range(8)],
                       ["mT", "Wout"], [pk])
                    tt("dve", xr[sl][:, hf * 512:(hf + 1) * 512], xr[sl][:, hf * 512:(hf + 1) * 512], pb, ALU.add,
                       ["xr%d" % sl, pk], ["xr%d" % sl])
                P.dma("sp", d_o[sl], x1s_d[i * 128:(i + 1) * 128, :], xr[sl], reads=["xr%d" % sl], writes=["x1d%d" % i])

        except _StopBuild:
            ng = 0
        skipB = lim < 99
        P.barrier()
        A.off = base_off
        Wg = A.alloc([8, DFF], BF16)
        Wu = A.alloc([8, DFF], BF16)
        Wd = A.alloc([NDC, D], BF16)
        x1t = [A.alloc([D], F32) for _ in range(NGT)]
        fn_ = [A.alloc([D], BF16) for _ in range(2)]
        fT = A.alloc([8, GT], BF16)
        actT = A.alloc([NDC, GT], BF16)
        slb = [A.alloc([GT], F32) for _ in range(2)]
        ot = [A.alloc([D], F32) for _ in range(1)]
        junkb = A.alloc([D], BF16)
        ss2 = A.alloc([NGT], F32)
        rstd2 = A.alloc([NGT], F32)
        ss3 = A.alloc([NGT], F32)
        rstd3 = A.alloc([NGT], F32)
        print("phase B arena bytes:", A.off)

        d_wb = P.dma_sem("d_wB")
        wg_v = wg_d.rearrange("(k p) n -> p k n", p=128)
        wu_v = wu_d.rearrange("(k p) n -> p k n", p=128)
        wd_v = wd_d.rearrange("(k p) n -> p k n", p=128)
        NWS = 4
        wsl = DFF // NWS
        for i in range(0 if skipB else NWS):
            P.dma("pool", None, Wg[:, :, i * wsl:(i + 1) * wsl], wg_v[:, :, i * wsl:(i + 1) * wsl], writes=["Wg%d" % i])
            P.dma("pool", None, Wu[:, :, i * wsl:(i + 1) * wsl], wu_v[:, :, i * wsl:(i + 1) * wsl], writes=["Wu%d" % i])
        for i in range(0 if skipB else 2):
            P.dma("pool", None, Wd[:, i * 11:(i + 1) * 11, :], wd_v[:, i * 11:(i + 1) * 11, :], writes=["Wd%d" % i])

        d_x1 = [P.dma_sem("d_x1_%d" % i) for i in range(NGT)]
        d_out = [P.dma_sem("d_out%d" % i) for i in range(1)]
        out_cnt = 0
        for g_ in range(ng):
            for c in range(NGT):
                i = g_ * NGT + c
                P.dma("sp", d_x1[c], x1t[c], x1s_d[i * 128:(i + 1) * 128, :], reads=["x1d%d" % i], writes=["x1t%d" % c])
                act(junkb, x1t[c], AF.Square, ["x1t%d" % c], ["junkb", "ss2_%d" % c], accum=ss2[:, c:c + 1])
            act(rstd2, ss2, AF.Ln, ["ss2_%d" % c for c in range(NGT)], ["rstd2"], scale=1.0 / D, bias=EPS)
            act(rstd2, rstd2, AF.Exp, ["rstd2"], ["rstd2"], scale=-0.5)
            for c in range(NGT):
                sl = c % 2
                stt(fn_[sl], x1t[c], rstd2[:, c:c + 1], g2rep, ALU.mult, ALU.mult,
                    ["x1t%d" % c, "rstd2", "cst2"], ["fn%d" % sl])
                transposes([(ptr_t[:, k * 128:(k + 1) * 128], fn_[sl][:, k * 128:(k + 1) * 128]) for k in range(8)],
                           identb, ["fn%d" % sl, "identb"], ["ptr"])
                cpy("act", fT[:, :, c * 128:(c + 1) * 128], ptr_t.rearrange("p (k t) -> p k t", k=8), ["ptr"], ["fT%d" % c])
            fTkeys = ["fT%d" % c for c in range(NGT)]
            for d_ in range(NDC):
                wks = sorted({d_ * 128 // wsl, (d_ * 128 + 127) // wsl})
                pg_, pgk = pbank[2 + (d_ % 2) * 2], "pbB%d" % (2 + (d_ % 2) * 2)
                pu_, puk = pbank[3 + (d_ % 2) * 2], "pbB%d" % (3 + (d_ % 2) * 2)
                mm(pg_, [(Wg[:, k, d_ * 128:(d_ + 1) * 128], fT[:, k, :]) for k in range(8)], fTkeys + ["Wg%d" % w_ for w_ in wks], [pgk])
                mm(pu_, [(Wu[:, k, d_ * 128:(d_ + 1) * 128], fT[:, k, :]) for k in range(8)], fTkeys + ["Wu%d" % w_ for w_ in wks], [puk])
                sb_ = slb[d_ % 2]
                sbk = "slb%d" % (d_ % 2)
                act(sb_, pg_, AF.Silu, [pgk], [sbk])
                tt("dve", actT[:, d_, :], sb_, pu_, ALU.mult, [sbk, puk], ["actT"])
            for c in range(NGT):
                i = g_ * NGT + c
                for hf in range(2):
                    pb, pk = pbank[hf], "pbB%d" % hf
                    mm(pb, [(actT[:, d_, c * 128:(c + 1) * 128], Wd[:, d_, hf * 512:(hf + 1) * 512]) for d_ in range(NDC)],
                       ["actT", "Wd0", "Wd1"], [pk])
                    tt("dve", x1t[c][:, hf * 512:(hf + 1) * 512], x1t[c][:, hf * 512:(hf + 1) * 512], pb, ALU.add,
                       ["x1t%d" % c, pk], ["x1t%d" % c])
                act(junkb, x1t[c], AF.Square, ["x1t%d" % c], ["junkb", "ss3_%d" % c], accum=ss3[:, c:c + 1])
            act(rstd3, ss3, AF.Ln, ["ss3_%d" % c for c in range(NGT)], ["rstd3"], scale=1.0 / D, bias=EPS)
            act(rstd3, rstd3, AF.Exp, ["rstd3"], ["rstd3"], scale=-0.5)
            for c in range(NGT):
                i = g_ * NGT + c
                sl = 0
                out_cnt += 1
                stt(ot[sl], x1t[c], rstd3[:, c:c + 1], gfrep, ALU.mult, ALU.mult,
                    ["x1t%d" % c, "rstd3", "cst2"], ["ot%d" % sl])
                P.dma("sp", d_out[sl], out_d[i * 128:(i + 1) * 128, :], ot[sl], reads=["ot%d" % sl, "x1t%d" % c],
                      writes=["outd%d" % i])
        P.ops["sp"].append(([(sname, P.dsem_cnt[sname]) for sname in d_out if P.dsem_cnt[sname] > 0], None, None))
        P.emit()
    return nc


def _win_perm():
    aq0, ak0, av0, mqk0, mv0, mo0, mi0, mf0, ga0, gm0 = np.cumsum([0, 512, 128, 128, 512, 512, 512, 4, 4, 1024])
    cols = []
    for j in range(4):
        cols += list(range(aq0 + j * 64, aq0 + (j + 1) * 64))
        cols += list(range(aq0 + (4 + j) * 64, aq0 + (5 + j) * 64))
    cols += list(range(ak0, ak0 + 128))
    cols += list(range(mqk0, mqk0 + 512))
    cols += list(range(ga0, ga0 + 1024))
    cols += list(range(gm0, gm0 + 1024))
    cols += list(range(mv0, mv0 + 512))
    cols += list(range(mo0, mo0 + 512))
    cols += list(range(av0, av0 + 128))
    cols += list(range(mi0, mi0 + 4))
    cols += list(range(mf0, mf0 + 4))
    assert len(cols) == INW
    return np.array(cols)


def _const_tables():
    idf = np.eye(128, dtype=np.float32)
    tri = np.triu(np.ones((128, 128), np.float32))
    slopes = np.exp2(-8.0 * np.arange(1, 9, dtype=np.float32) / 8).astype(np.float32)
    k = np.arange(128)[:, None]
    q = np.arange(128)[None, :]
    E = np.zeros((128, 2, 2, 4, 128), np.float32)
    for g in range(2):
        for j in range(4):
            sl = slopes[4 * g + j]
            d0 = (q - k + 128).astype(np.float32)
            E[:, 0, g, j, :] = np.where(k > q, np.exp(-sl * d0), 0.0)
            d1 = (q - k).astype(np.float32)
            E[:, 1, g, j, :] = np.where(k <= q, np.exp(-sl * d1), 0.0)
    onesA = np.zeros((128, 2, 128), np.float32)
    onesA[:, 0, 0:64] = 1.0
    onesA[:, 1, 64:128] = 1.0
    return idf, tri, E.reshape(128, 2048), onesA.reshape(128, 256)


_NC_CACHE = {}


def kernel(x, norm1_g, w_in, conv_w, conv_b, i_bias, f_bias, mlstm_norm_g, attn_sinks,
           w_attn_branch, w_mlstm_branch, w_out, norm2_g, w_ffn_gate, w_ffn_up, w_ffn_down,
           final_norm_g):
    f32 = np.float32
    x = np.asarray(x, f32)
    B = x.shape[0]
    perm = _win_perm()
    w_in_p = np.ascontiguousarray(np.asarray(w_in, f32)[0][:, perm])
    wab = np.asarray(w_attn_branch, f32)[0]
    rows = []
    for j in range(4):
        rows += list(range(j * 64, (j + 1) * 64)) + list(range((4 + j) * 64, (5 + j) * 64))
    wab_p = np.ascontiguousarray(wab[np.array(rows)])
    idf, tri, E, onesA = _const_tables()
    cst = np.zeros((128, K_END), f32)
    cst[:, K_IDF:K_IDF + 128] = idf
    cst[:, K_TRI:K_TRI + 128] = tri
    cst[:, K_G1:K_G1 + 1024] = np.asarray(norm1_g, f32)[0][None, :]
    cst[:, K_GM:K_GM + 512] = np.asarray(mlstm_norm_g, f32)[0][None, :]
    cst[:, K_B8:K_B8 + 4] = np.asarray(i_bias, f32)[0][None, :]
    cst[:, K_B8 + 4:K_B8 + 8] = np.asarray(f_bias, f32)[0][None, :]
    cw = np.asarray(conv_w, f32)[0]
    cst[:, K_CW:K_CW + 16] = cw.reshape(4, 4, 128).transpose(2, 1, 0).reshape(128, 16)
    cst[:, K_CB:K_CB + 4] = np.asarray(conv_b, f32)[0].reshape(4, 128).T
    cst[:, K_ONE:K_ONE + 128] = 1.0
    cstb = np.zeros((128, KB_END), f32)
    cstb[:, KB_E:KB_E + 2048] = E
    cstb[:, KB_ONA:KB_ONA + 256] = onesA
    cstb[:, KB_IDB:KB_IDB + 128] = idf
    sk = np.asarray(attn_sinks, f32)[0]
    snk = np.zeros((128, 512), f32)
    snk[0:64, :] = np.repeat(sk[0:4], 128)[None, :]
    snk[64:128, :] = np.repeat(sk[4:8], 128)[None, :]
    cst2 = np.zeros((128, 2048), f32)
    cst2[:, 0:1024] = np.asarray(norm2_g, f32)[0][None, :]
    cst2[:, 1024:2048] = np.asarray(final_norm_g, f32)[None, :]

    if "nc" not in _NC_CACHE:
        _NC_CACHE["nc"] = build_program()
    nc = _NC_CACHE["nc"]
    shared = {
        "w_in": w_in_p, "w_ab": wab_p,
        "w_mb": np.ascontiguousarray(np.asarray(w_mlstm_branch, f32)[0]),
        "w_out": np.ascontiguousarray(np.asarray(w_out, f32)[0]),
        "w_g": np.ascontiguousarray(np.asarray(w_ffn_gate, f32)[0]),
        "w_u": np.ascontiguousarray(np.asarray(w_ffn_up, f32)[0]),
        "w_d": np.ascontiguousarray(np.asarray(w_ffn_down, f32)[0]),
        "cst": cst, "cstb": cstb, "snk": snk, "cst2": cst2,
    }
    in_maps = []
    for b in range(B):
        m = dict(shared)
        m["x"] = np.ascontiguousarray(x[b])
        in_maps.append(m)
    res = run_bass_kernel_spmd(nc, in_maps, core_ids=list(range(B)))
    out = np.stack([np.asarray(r["out"], dtype=f32) for r in res.results], axis=0)
    return out
```
